# Optimizing a Trainium2 kernel written in Bass

```python
import jax, jax.numpy as jnp
from jax import lax
import numpy as np

D_MODEL = 1024
BATCH = 2
SEQ = 8192
DEPTH = 2

CHUNK = 64
EPS = 1e-6
ROPE_THETA = 10000.0
N_BRANCH = 3

GM_GROUPS = 4
GM_GROUP_DIM = 128
GM_WIDTH = GM_GROUPS * GM_GROUP_DIM
GM_BLOCK = 128

ATT_HEADS = 4
ATT_HEAD_DIM = 128
ATT_WIDTH = ATT_HEADS * ATT_HEAD_DIM
IDX_HEADS = 8
IDX_DIM = 64
TOPK_MAX = 256
Q_BLOCK = 128

LRU_BLOCKS = 8
LRU_BLOCK_DIM = 64
LRU_WIDTH = LRU_BLOCKS * LRU_BLOCK_DIM
LRU_CONV = 4
LRU_C = 8.0

FFN_DIM = 2816
FFN_CONV = 3

IN_SIZES = (GM_WIDTH, GM_WIDTH, ATT_WIDTH, ATT_WIDTH, ATT_WIDTH,
            IDX_HEADS * IDX_DIM, IDX_DIM, IDX_HEADS, LRU_WIDTH, LRU_WIDTH)
IN_SPLITS = tuple(int(v) for v in np.cumsum(IN_SIZES)[:-1])
D_IN = int(sum(IN_SIZES))
BRANCH_WIDTH = 512

kernel_name = 'hybrid_gmlp_dsa_rglru_convffn_block'


def rms_norm(x, g):
    xf = x.astype(jnp.float32)
    y = xf * lax.rsqrt(jnp.mean(xf * xf, axis=-1, keepdims=True) + EPS)
    return (y * g).astype(x.dtype)


def layer_norm(x, g):
    xf = x.astype(jnp.float32)
    mu = jnp.mean(xf, axis=-1, keepdims=True)
    xc = xf - mu
    var = jnp.mean(xc * xc, axis=-1, keepdims=True)
    return (xc * lax.rsqrt(var + EPS) * g).astype(x.dtype)


def rope_tables(seq, dim):
    inv = 1.0 / (ROPE_THETA ** (jnp.arange(0, dim, 2, dtype=jnp.float32) / dim))
    ang = jnp.arange(seq, dtype=jnp.float32)[:, None] * inv[None, :]
    return jnp.cos(ang), jnp.sin(ang)


def apply_rope(x, cos, sin):
    x1, x2 = jnp.split(x.astype(jnp.float32), 2, axis=-1)
    cs = cos[None, :, None, :]
    sn = sin[None, :, None, :]
    return jnp.concatenate([x1 * cs - x2 * sn, x2 * cs + x1 * sn], axis=-1).astype(x.dtype)


def causal_dwconv(x, w, b):
    kw = w.shape[0]
    y = lax.conv_general_dilated(
        x, w[:, None, :].astype(x.dtype), window_strides=(1,), padding=[(kw - 1, 0)],
        dimension_numbers=('NWC', 'WIO', 'NWC'), feature_group_count=x.shape[-1])
    return y + b


def gmlp_spatial_gate(u, v, g_v, w_s, b_s):
    bsz, seq, _ = u.shape
    v = layer_norm(v, g_v)
    nb = seq // GM_BLOCK
    v = v.reshape(bsz, nb, GM_BLOCK, GM_GROUPS, GM_GROUP_DIM)
    pos = jnp.arange(GM_BLOCK)
    mask = (pos[:, None] // CHUNK) >= (pos[None, :] // CHUNK)
    w = jnp.where(mask[None], w_s, 0.0)
    mixed = jnp.einsum('gts,bnsgc->bntgc', w, v) + b_s.T[None, None, :, :, None]
    return u * mixed.reshape(bsz, seq, GM_WIDTH)


def dsa_attention(q, k, v, qi, ki, wi, topk):
    bsz, seq = q.shape[0], q.shape[1]
    n_qb = seq // Q_BLOCK
    key_chunk = jnp.arange(seq) // CHUNK

    def one_block(i):
        start = i * Q_BLOCK
        qb = lax.dynamic_slice_in_dim(q, start, Q_BLOCK, axis=1)
        qib = lax.dynamic_slice_in_dim(qi, start, Q_BLOCK, axis=1)
        wib = lax.dynamic_slice_in_dim(wi, start, Q_BLOCK, axis=1)
        q_chunk = (start + jnp.arange(Q_BLOCK)) // CHUNK
        logits = jnp.einsum('bqhd,bsd->bqhs', qib, ki).astype(jnp.float32) * (IDX_DIM ** -0.5)
        score = jnp.einsum('bqhs,bqh->bqs', jax.nn.relu(logits),
                           wib.astype(jnp.float32)) * (IDX_HEADS ** -0.5)
        admissible = key_chunk[None, :] <= q_chunk[:, None]
        score = jnp.where(admissible[None], score, -jnp.inf)
        _, idx = lax.top_k(score, topk)
        valid = (idx // CHUNK) <= q_chunk[None, :, None]
        kg = jax.vmap(lambda kb, ib: kb[ib])(k, idx)
        vg = jax.vmap(lambda vb, ib: vb[ib])(v, idx)
        s = jnp.einsum('bqhd,bqkhd->bhqk', qb, kg).astype(jnp.float32) * (ATT_HEAD_DIM ** -0.5)
        s = jnp.where(valid[:, None], s, -jnp.inf)
        p = jax.nn.softmax(s, axis=-1).astype(v.dtype)
        o = jnp.einsum('bhqk,bqkhd->bqhd', p, vg)
        return o.reshape(bsz, Q_BLOCK, ATT_WIDTH)

    out = lax.map(one_block, jnp.arange(n_qb))
    return out.transpose(1, 0, 2, 3).reshape(bsz, seq, ATT_WIDTH)


def rg_lru_branch(xb, gate, conv_w, conv_b, w_r, b_r, w_i, b_i, lam):
    bsz, seq, _ = xb.shape
    xc = causal_dwconv(xb, conv_w, conv_b)
    xg = xc.reshape(bsz, seq, LRU_BLOCKS, LRU_BLOCK_DIM)
    r = jax.nn.sigmoid(jnp.einsum('bsnc,ncd->bsnd', xg, w_r).reshape(bsz, seq, LRU_WIDTH) + b_r)
    i = jax.nn.sigmoid(jnp.einsum('bsnc,ncd->bsnd', xg, w_i).reshape(bsz, seq, LRU_WIDTH) + b_i)
    log_a = -LRU_C * r.astype(jnp.float32) * jax.nn.softplus(-lam.astype(jnp.float32))
    a = jnp.exp(log_a)
    b = jnp.sqrt(-jnp.expm1(2.0 * log_a)) * (i * xc).astype(jnp.float32)

    def combine(left, right):
        a1, b1 = left
        a2, b2 = right
        return a1 * a2, a2 * b1 + b2

    _, h = lax.associative_scan(combine, (a, b), axis=1)
    return h.astype(xb.dtype) * jax.nn.gelu(gate)


def conv_ffn(h, w_up, conv_w, conv_b, w_down):
    z = causal_dwconv(h @ w_up, conv_w, conv_b)
    g, val = jnp.split(z, 2, axis=-1)
    return (jax.nn.silu(g) * val) @ w_down


def setup_inputs(seed: int = 0) -> dict:
    key = jax.random.key(seed)
    ks = jax.random.split(key, 32)
    f32 = jnp.float32

    def nrm(k, shape, scale):
        return scale * jax.random.normal(k, shape, f32)

    def gain(k, shape, s=0.05):
        return 1.0 + s * jax.random.normal(k, shape, f32)

    p = jax.random.uniform(ks[17], (DEPTH, LRU_WIDTH), f32, 0.9, 0.999) ** (1.0 / LRU_C)
    lam = jnp.log(p) - jnp.log1p(-p)
    return {
        'x': nrm(ks[0], (BATCH, SEQ, D_MODEL), 1.0),
        'c': nrm(ks[1], (BATCH, D_MODEL), 1.0),
        'w_mod': nrm(ks[2], (DEPTH, D_MODEL, 6 * D_MODEL), 0.5 * D_MODEL ** -0.5),
        'b_mod': nrm(ks[3], (DEPTH, 6 * D_MODEL), 0.01),
        'g_pre1': gain(ks[4], (DEPTH, D_MODEL)),
        'g_post1': gain(ks[5], (DEPTH, D_MODEL)),
        'w_in': nrm(ks[6], (DEPTH, D_MODEL, D_IN), D_MODEL ** -0.5),
        'gm_v_g': gain(ks[7], (DEPTH, GM_WIDTH)),
        'gm_w_s': nrm(ks[8], (DEPTH, GM_GROUPS, GM_BLOCK, GM_BLOCK), GM_BLOCK ** -0.5),
        'gm_b_s': gain(ks[9], (DEPTH, GM_GROUPS, GM_BLOCK), 0.1),
        'lru_conv_w': nrm(ks[10], (DEPTH, LRU_CONV, LRU_WIDTH), LRU_CONV ** -0.5),
        'lru_conv_b': nrm(ks[11], (DEPTH, LRU_WIDTH), 0.01),
        'lru_w_r': nrm(ks[12], (DEPTH, LRU_BLOCKS, LRU_BLOCK_DIM, LRU_BLOCK_DIM), LRU_BLOCK_DIM ** -0.5),
        'lru_b_r': nrm(ks[13], (DEPTH, LRU_WIDTH), 0.01),
        'lru_w_i': nrm(ks[14], (DEPTH, LRU_BLOCKS, LRU_BLOCK_DIM, LRU_BLOCK_DIM), LRU_BLOCK_DIM ** -0.5),
        'lru_b_i': nrm(ks[15], (DEPTH, LRU_WIDTH), 0.01),
        'lru_lam': lam,
        'w_branch': nrm(ks[18], (DEPTH, N_BRANCH, BRANCH_WIDTH, D_MODEL), BRANCH_WIDTH ** -0.5),
        'w_gate': nrm(ks[19], (DEPTH, D_MODEL, N_BRANCH * D_MODEL), D_MODEL ** -0.5),
        'b_gate': nrm(ks[20], (DEPTH, N_BRANCH * D_MODEL), 0.01),
        'w_o': nrm(ks[21], (DEPTH, D_MODEL, D_MODEL), D_MODEL ** -0.5),
        'g_pre2': gain(ks[22], (DEPTH, D_MODEL)),
        'g_post2': gain(ks[23], (DEPTH, D_MODEL)),
        'ffn_w_up': nrm(ks[24], (DEPTH, D_MODEL, 2 * FFN_DIM), D_MODEL ** -0.5),
        'ffn_conv_w': nrm(ks[25], (DEPTH, FFN_CONV, 2 * FFN_DIM), FFN_CONV ** -0.5),
        'ffn_conv_b': nrm(ks[26], (DEPTH, 2 * FFN_DIM), 0.01),
        'ffn_w_down': nrm(ks[27], (DEPTH, FFN_DIM, D_MODEL), FFN_DIM ** -0.5),
    }


def reference(x, c, w_mod, b_mod, g_pre1, g_post1, w_in, gm_v_g, gm_w_s, gm_b_s,
              lru_conv_w, lru_conv_b, lru_w_r, lru_b_r, lru_w_i, lru_b_i, lru_lam,
              w_branch, w_gate, b_gate, w_o, g_pre2, g_post2,
              ffn_w_up, ffn_conv_w, ffn_conv_b, ffn_w_down):
    bsz, seq, _ = x.shape
    topk = min(TOPK_MAX, seq // 4)
    cos_a, sin_a = rope_tables(seq, ATT_HEAD_DIM)
    cos_i, sin_i = rope_tables(seq, IDX_DIM)
    c_act = jax.nn.silu(c)
    for l in range(DEPTH):
        mod = c_act @ w_mod[l] + b_mod[l]
        sh1, sc1, gt1, sh2, sc2, gt2 = jnp.split(mod[:, None, :], 6, axis=-1)

        h = rms_norm(x, g_pre1[l]) * (1.0 + sc1) + sh1
        gu, gv, q, k, v, qi, ki, wi, lx, lg = jnp.split(h @ w_in[l], IN_SPLITS, axis=-1)

        y_a = gmlp_spatial_gate(jax.nn.gelu(gu), jax.nn.gelu(gv), gm_v_g[l], gm_w_s[l], gm_b_s[l])

        q = apply_rope(q.reshape(bsz, seq, ATT_HEADS, ATT_HEAD_DIM), cos_a, sin_a)
        k = apply_rope(k.reshape(bsz, seq, ATT_HEADS, ATT_HEAD_DIM), cos_a, sin_a)
        v = v.reshape(bsz, seq, ATT_HEADS, ATT_HEAD_DIM)
        qi = apply_rope(qi.reshape(bsz, seq, IDX_HEADS, IDX_DIM), cos_i, sin_i)
        ki = apply_rope(ki[:, :, None, :], cos_i, sin_i)[:, :, 0, :]
        y_b = dsa_attention(q, k, v, qi, ki, wi, topk)

        y_c = rg_lru_branch(lx, lg, lru_conv_w[l], lru_conv_b[l], lru_w_r[l], lru_b_r[l],
                            lru_w_i[l], lru_b_i[l], lru_lam[l])

        g_a, g_b, g_c = jnp.split(jax.nn.sigmoid(h @ w_gate[l] + b_gate[l]), N_BRANCH, axis=-1)
        merged = (g_a * (y_a @ w_branch[l, 0]) + g_b * (y_b @ w_branch[l, 1])
                  + g_c * (y_c @ w_branch[l, 2]))
        x = x + gt1 * rms_norm(merged @ w_o[l], g_post1[l])

        h2 = rms_norm(x, g_pre2[l]) * (1.0 + sc2) + sh2
        f = conv_ffn(h2, ffn_w_up[l], ffn_conv_w[l], ffn_conv_b[l], ffn_w_down[l])
        x = x + gt2 * rms_norm(f, g_post2[l])
    return x
```

```python
import math
from contextlib import ExitStack

import numpy as np
import concourse.bass as bass
import concourse.mybir as mybir
from concourse.bass_utils import run_bass_kernel_spmd

F32 = mybir.dt.float32
BF16 = mybir.dt.bfloat16
AF = mybir.ActivationFunctionType
ALU = mybir.AluOpType
AX = mybir.AxisListType

D = 1024
KC = 8
DEPTH = 2
T = 512
EPS = 1e-6
FFN = 2816
NPAIR = 22
NV = 316
NEG = -1.0e30
NIT = 14
OFF = dict(gu=0, gv=512, q=1024, k=1536, v=2048, qi=2560, ki=3072, wi=3136, lx=3144, lg=3656)

SEM_WINDOW = 50000
N_DMA_SEMS = 24


class Tile:
    def __init__(self, base, lo, hi, ap):
        self.base = base
        self.lo = lo
        self.hi = hi
        self.ap = ap
        self.last_write = None
        self.reads = {}

    def __getitem__(self, idx):
        return self.ap[idx]


class _Op:
    __slots__ = ("fn", "waits", "inc")

    def __init__(self, fn, waits, inc):
        self.fn = fn
        self.waits = waits
        self.inc = inc


class Sched:
    ENGS = ("pe", "act", "dve", "pool", "sp")

    def __init__(self, nc, stack):
        self.nc = nc
        self.stack = stack
        self.ops = {e: [] for e in self.ENGS}
        self.count = {e: 0 for e in self.ENGS}
        self.waited = {e: {} for e in self.ENGS}
        self.eng_sems = {}
        self.dma_sems = [stack.enter_context(nc.semaphore(f"dq{k}")) for k in range(2 * N_DMA_SEMS)]
        self.dma_val = [0] * (2 * N_DMA_SEMS)
        self.dma_rr = {"sp": 0, "pool": 0}
        self.bases = {}

    def sb(self, name, shape, dtype):
        t = self.stack.enter_context(self.nc.sbuf_tensor(name, list(shape), dtype))
        return self.reg(Tile(name, 0, 1, t[:]))

    def ps(self, name, shape, dtype):
        t = self.stack.enter_context(self.nc.psum_tensor(name, list(shape), dtype))
        return self.reg(Tile(name, 0, 1, t[:]))

    def view(self, base, lo, hi, ap):
        return self.reg(Tile(base, lo, hi, ap))

    def reg(self, t):
        self.bases.setdefault(t.base, []).append(t)
        return t

    def _overlaps(self, t):
        return [o for o in self.bases[t.base] if o.lo < t.hi and t.lo < o.hi]

    def _sem_for(self, eng, idx):
        w = (idx - 1) // SEM_WINDOW
        key = (eng, w)
        if key not in self.eng_sems:
            self.eng_sems[key] = self.stack.enter_context(self.nc.semaphore(f"s_{eng}_{w}"))
        return self.eng_sems[key], (idx - 1) % SEM_WINDOW + 1

    def _need(self, eng, dep, waits):
        if dep is None:
            return
        if dep[0] == "eng":
            _, e2, idx = dep
            if e2 == eng and eng == "pe":
                return
            key = ("eng", e2)
            if self.waited[eng].get(key, 0) >= idx:
                return
            waits[key] = max(waits.get(key, 0), idx)
        else:
            _, k, val = dep
            key = ("dma", k)
            if self.waited[eng].get(key, 0) >= val:
                return
            waits[key] = max(waits.get(key, 0), val)

    def _collect(self, eng, reads, writes):
        waits = {}
        for t in reads:
            for o in self._overlaps(t):
                self._need(eng, o.last_write, waits)
        for t in writes:
            for o in self._overlaps(t):
                self._need(eng, o.last_write, waits)
                for dep in o.reads.values():
                    self._need(eng, dep, waits)
        out = []
        for key, val in waits.items():
            self.waited[eng][key] = val
            if key[0] == "eng":
                out.append(self._sem_for(key[1], val))
            else:
                out.append((self.dma_sems[key[1]], val))
        return out

    def op(self, eng, fn, reads=(), writes=()):
        waits = self._collect(eng, reads, writes)
        self.count[eng] += 1
        idx = self.count[eng]
        sem, _ = self._sem_for(eng, idx)
        self.ops[eng].append(_Op(fn, waits, (sem, 1)))
        dep = ("eng", eng, idx)
        for t in writes:
            t.last_write = dep
            t.reads = {}
        for t in reads:
            if t not in writes:
                t.reads[("eng", eng)] = dep

    def dma(self, eng, fn, reads=(), writes=()):
        k = self.dma_rr[eng] + (N_DMA_SEMS if eng == "pool" else 0)
        self.dma_rr[eng] = (self.dma_rr[eng] + 1) % N_DMA_SEMS
        waits = self._collect(eng, reads, writes)
        prev = self.dma_val[k]
        key = ("dma", k)
        if prev and self.waited[eng].get(key, 0) < prev:
            self.waited[eng][key] = prev
            waits.append((self.dma_sems[k], prev))
        self.dma_val[k] += 16
        val = self.dma_val[k]
        self.ops[eng].append(_Op(fn, waits, (self.dma_sems[k], 16)))
        dep = ("dma", k, val)
        for t in writes:
            t.last_write = dep
            t.reads = {}
        for t in reads:
            t.reads[("dma", k)] = dep

    def finish(self, eng="sp"):
        waits = []
        for k in range(2 * N_DMA_SEMS):
            if self.dma_val[k] and self.waited[eng].get(("dma", k), 0) < self.dma_val[k]:
                waits.append((self.dma_sems[k], self.dma_val[k]))
        for e in self.ENGS:
            if e != eng and self.count[e]:
                waits.append(self._sem_for(e, self.count[e]))
        self.ops[eng].append(_Op(None, waits, None))

    def emit(self):
        ops = self.ops

        def run(e, lst):
            for o in lst:
                for sem, v in o.waits:
                    e.wait_ge(sem, v)
                if o.fn is not None:
                    ins = o.fn(e)
                    if o.inc is not None:
                        ins.then_inc(o.inc[0], o.inc[1])

        with self.nc.Block() as block:
            @block.sync
            def _(e):
                run(e, ops["sp"])

            @block.tensor
            def _(e):
                run(e, ops["pe"])

            @block.scalar
            def _(e):
                run(e, ops["act"])

            @block.vector
            def _(e):
                run(e, ops["dve"])

            @block.gpsimd
            def _(e):
                run(e, ops["pool"])


WSPEC = {
    "wF": (9, 8, 512), "wF9": (1, 8, 256), "wgv": (1, 8, 512), "wv": (1, 8, 512),
    "wg": (8, 8, 384), "wb": (4, 12, 256), "wo": (2, 8, 512), "wu": (11, 8, 512),
    "wd": (8, 22, 128), "wm": (12, 8, 512),
}


class Prog:
    def __init__(self, S_len, topk):
        self.S_len = S_len
        self.NSB = S_len // T
        self.topk = topk
        self.nc = bass.Bass("TRN2", target_bir_lowering=False)
        self.stack = ExitStack()
        self.S = Sched(self.nc, self.stack)
        self.rr = {}
        self.debug = False
        self.dbg_names = []

    def rot(self, name, lst):
        i = self.rr.get(name, 0)
        self.rr[name] = i + 1
        return lst[i % len(lst)]

    def act(self, out, in_, func, reads, writes, **kw):
        self.S.op("act", lambda e: e.activation(out=out, in_=in_, func=func, **kw), reads, writes)

    def tt(self, eng, out, in0, in1, op, reads, writes):
        self.S.op(eng, lambda e: e.tensor_tensor(out=out, in0=in0, in1=in1, op=op), reads, writes)

    def ts(self, eng, out, in0, s1, s2, op0, op1, reads, writes, **kw):
        if op1 is None:
            self.S.op(eng, lambda e: e.tensor_scalar(out=out, in0=in0, scalar1=s1, scalar2=None, op0=op0, **kw), reads, writes)
        else:
            self.S.op(eng, lambda e: e.tensor_scalar(out=out, in0=in0, scalar1=s1, scalar2=s2, op0=op0, op1=op1, **kw), reads, writes)

    def stt(self, out, in0, scalar, in1, op0, op1, reads, writes):
        self.S.op("dve", lambda e: e.scalar_tensor_tensor(out=out, in0=in0, scalar=scalar, in1=in1, op0=op0, op1=op1), reads, writes)

    def mm(self, out, lhsT, rhs, start, stop, reads, writes, skip=False):
        self.S.op("pe", lambda e: e.matmul(out, lhsT=lhsT, rhs=rhs, start=start, stop=stop, skip_group_check=skip), reads, writes)

    def tr(self, out, in_, reads, writes):
        ident = self.identb.ap
        self.S.op("pe", lambda e: e.transpose(out, in_, ident), list(reads) + [self.identb], writes)

    def copy(self, eng, out, in_, reads, writes):
        if eng == "act":
            self.S.op("act", lambda e: e.activation(out=out, in_=in_, func=AF.Copy), reads, writes)
        else:
            self.S.op(eng, lambda e: e.tensor_copy(out=out, in_=in_), reads, writes)

    def memset(self, eng, tile, ap, val):
        self.S.op(eng, lambda e: e.memset(ap, val), (), [tile])

    def next_ps(self):
        return self.rot("ps", self.pspool)

    def dump(self, name, tile, ap=None, dtype=F32):
        if not getattr(self, "debug", False):
            return
        ap = tile.ap if ap is None else ap
        shp = list(ap.shape)
        name = f"{name}_L{self.cur_l}"
        d = self.nc.dram_tensor("dbg_" + name, shp, dtype, kind="ExternalOutput").ap()
        self.dbg_names.append("dbg_" + name)
        self.S.dma("sp", lambda e: e.dma_start(out=d, in_=ap), [tile], ())

    def declare(self):
        nc, S, S_len = self.nc, self.S, self.S_len
        dt = nc.dram_tensor
        self.xT = dt("xT", [D, S_len], F32, kind="ExternalInput").ap()
        self.outT = dt("outT", [D, S_len], F32, kind="ExternalOutput").ap()
        self.cT = dt("cT", [128, 8], F32, kind="ExternalInput").ap()
        self.vecd = dt("vec", [DEPTH, 128, NV], F32, kind="ExternalInput").ap()
        self.src = {}
        shapes = {"wF": [D, 9 * 512], "wF9": [D, 256], "wgv": [D, 512], "wv": [D, 512], "wg": [D, 8 * 384],
                  "wb": [1536, 1024], "wo": [D, D], "wu": [D, 2 * FFN], "wd": [FFN, D], "wm": [D, 6 * D]}
        for k, shp in shapes.items():
            self.src[k] = dt("s_" + k, [DEPTH] + shp, F32, kind="ExternalInput").ap()
        self.wwi_d = dt("s_wwi", [DEPTH, D, 8], F32, kind="ExternalInput").ap()
        self.bd_d = dt("s_bd", [DEPTH, 2, 4, 128, 128], F32, kind="ExternalInput").ap()
        self.wsT_d = dt("s_wsT", [DEPTH, 4, 128, 128], F32, kind="ExternalInput").ap()
        self.bs_d = dt("s_bs", [DEPTH, 4, 128], F32, kind="ExternalInput").ap()
        self.rope_d = dt("rope", [4, 128, S_len], F32, kind="ExternalInput").ap()
        self.cst_d = dt("cst", [128, 256 + NIT + 1], F32, kind="ExternalInput").ap()
        self.wsc = {}
        self.wsc_t = {}
        for l in range(DEPTH):
            for k, (npn, kc, pw) in WSPEC.items():
                nm = f"w_{k}_{l}"
                self.wsc[(k, l)] = dt(nm, [npn, 128, kc, pw], BF16, kind="Internal").ap()
                self.wsc_t[(k, l)] = [S.view(nm, pn, pn + 1, None) for pn in range(npn)]
        self.kTc, self.Vc, self.kic = [], [], []
        self.kTc_t, self.Vc_t, self.kic_t = [], [], []
        for l in range(DEPTH):
            self.kTc.append(dt(f"kTc{l}", [4, 128, S_len], BF16, kind="Internal").ap())
            self.Vc.append(dt(f"Vc{l}", [S_len // 128, 128, 516], BF16, kind="Internal").ap())
            self.kic.append(dt(f"kic{l}", [128, S_len], BF16, kind="Internal").ap())
            self.kTc_t.append([S.view(f"kTc{l}", g, g + 1, None) for g in range(self.NSB)])
            self.Vc_t.append([S.view(f"Vc{l}", g, g + 1, None) for g in range(self.NSB * 4)])
            self.kic_t.append([S.view(f"kic{l}", g, g + 1, None) for g in range(self.NSB)])

        sb = S.sb
        self.x = [sb(f"x{j}", [128, T], F32) for j in range(KC)]
        self.hT = [sb(f"hT{j}", [128, T], BF16) for j in range(KC)]
        self.wbuf = [sb(f"wbuf{i}", [128, 4096], BF16) for i in range(3)]
        self.qT = [sb(f"qT{h}", [128, T], BF16) for h in range(4)]
        self.qiZ = [sb(f"qiZ{h}", [128, T], BF16) for h in range(8)]
        self.ya = [sb(f"ya{g}", [128, T], BF16) for g in range(4)]
        self.yb = sb("yb", [128, 4 * T], BF16)
        self.yc = [sb(f"yc{g}", [128, T], BF16) for g in range(4)]
        self.rope = [sb(f"rope{i}", [128, T], F32) for i in range(4)]
        self.cst = sb("cstf", [128, 256 + NIT + 1], F32)
        self.identb = sb("identb", [128, 128], BF16)
        self.onesb = sb("onesb", [128, 128], BF16)
        self.vec = [sb(f"vec{l}", [128, NV], F32) for l in range(DEPTH)]
        self.mod = [sb(f"mod{l}", [128, 48], F32) for l in range(DEPTH)]
        self.A1 = [sb(f"A1_{l}", [128, 8], F32) for l in range(DEPTH)]
        self.A2 = [sb(f"A2_{l}", [128, 8], F32) for l in range(DEPTH)]
        self.G1 = [sb(f"G1_{l}", [128, 8], F32) for l in range(DEPTH)]
        self.G2 = [sb(f"G2_{l}", [128, 8], F32) for l in range(DEPTH)]
        self.nsp = [sb(f"nsp{l}", [128, 8], F32) for l in range(DEPTH)]
        self.sc_b = sb("sc_b", [128, 8], BF16)
        self.small = sb("small", [128, 64], F32)
        self.wwi = [sb(f"wwi{l}", [128, 64], BF16) for l in range(DEPTH)]
        self.bd = [sb(f"bd{l}", [128, 8 * 128], BF16) for l in range(DEPTH)]
        self.wsm = [sb(f"wsm{l}", [128, 4 * 128], BF16) for l in range(DEPTH)]
        self.bbc = [sb(f"bbc{l}", [128, 4 * 128], F32) for l in range(DEPTH)]
        self.lxh = [sb(f"lxh{l}", [128, 4 * 3], F32) for l in range(DEPTH)]
        self.lst = [sb(f"lst{l}", [128, 4], F32) for l in range(DEPTH)]
        self.zh = [sb(f"zh{l}", [128, 44 * 2], F32) for l in range(DEPTH)]
        self.scr = [sb(f"scr{i}", [128, T], F32) for i in range(8)]
        self.rstd = sb("rstd", [128, T], F32)
        self.sqb = [sb(f"sqb{i}", [128, T], BF16) for i in range(2)]
        self.wiS = [sb(f"wiS{s}", [128, 8], F32) for s in range(4)]
        self.st6 = sb("st6", [128, 8], F32)
        self.midt = sb("midt", [128, 1], F32)
        self.cntA = sb("cntA", [128, 1], F32)
        self.cntB = sb("cntB", [128, 1], F32)
        self.cntT = sb("cntT", [128, 2], F32)
        self.Vsb = sb("Vsb", [128, 516], BF16)
        SC = self.S_len
        nuf = max(SC + 3 * T, 8 * T + 4 * 514, 4 * 515)
        nub = max(SC + 2048 + 2 * 2048 + 2 * 2048 + 2 * 2064 + 7 * T, 4 * T + NPAIR * T, 10768)
        uf = self.stack.enter_context(nc.sbuf_tensor("UF", [128, nuf], F32))
        ub = self.stack.enter_context(nc.sbuf_tensor("UB", [128, nub], BF16))

        def carve(base, t, start, n):
            return S.view(base, start, start + n, t[:, start:start + n])

        self.score = carve("UF", uf, 0, SC)
        self.scoreblk = [carve("UF", uf, c, min(T, SC - c)) for c in range(0, SC, T)]
        self.relu = [carve("UF", uf, SC + i * T, T) for i in range(3)]
        self.o = [carve("UF", uf, j * T, T) for j in range(KC)]
        self.zb = [carve("UF", uf, 8 * T + i * 514, 514) for i in range(4)]
        self.lxb = [carve("UF", uf, i * 515, 515) for i in range(4)]
        o_ = 0
        self.mask = carve("UB", ub, o_, SC); o_ += SC
        self.maskTg = [carve("UB", ub, o_ + i * 1024, 1024) for i in range(2)]
        o_ += 2048
        self.kig = [carve("UB", ub, o_ + i * 2048, 2048) for i in range(2)]; o_ += 4096
        self.kTg = [carve("UB", ub, o_ + i * 2048, 2048) for i in range(2)]; o_ += 4096
        self.Vg = [carve("UB", ub, o_ + i * 2064, 2064) for i in range(2)]; o_ += 4128
        self.pexp = [carve("UB", ub, o_ + i * T, T) for i in range(2)]; o_ += 2 * T
        self.pm = [carve("UB", ub, o_ + i * T, T) for i in range(4)]; o_ += 4 * T
        self.onb = carve("UB", ub, o_, T); o_ += T
        assert o_ <= nub
        self.mg = [carve("UB", ub, j * T, T) for j in range(KC)]
        self.actb = [carve("UB", ub, 8 * T + c * T, T) for c in range(NPAIR)]
        o_ = 0
        self.u = [carve("UB", ub, o_ + g * T, T) for g in range(4)]; o_ += 4 * T
        self.glg = [carve("UB", ub, o_ + g * T, T) for g in range(4)]; o_ += 4 * T
        self.vh = [carve("UB", ub, o_ + s * T, T) for s in range(4)]; o_ += 4 * T
        self.kTsb = carve("UB", ub, o_, 4 * T); o_ += 4 * T
        self.kiTsb = carve("UB", ub, o_, T); o_ += T
        self.xcb = carve("UB", ub, o_, T); o_ += T
        assert o_ <= nub
        self.pspool = [S.ps(f"ps{i}", [128, 512], F32) for i in range(5)]
        self.psO = [S.ps(f"psO{i}", [128, 512], F32) for i in range(2)]
        self.psN = self.psO[0]
        self.psb = S.ps("psb", [128, 1024], BF16)

    def precast(self, l):
        S = self.S
        for k, (npn, kc, pw) in WSPEC.items():
            for pn in range(npn):
                src = self.src[k][l][:, pn * pw:(pn + 1) * pw].rearrange("(kc p) n -> p kc n", p=128)
                dst = self.wsc[(k, l)][pn]
                S.dma("pool", lambda e, s=src, d=dst: e.dma_start(out=d, in_=s), (), [self.wsc_t[(k, l)][pn]])

    def panel(self, k, l, pn):
        npn, kc, pw = WSPEC[k]
        wb = self.rot("wbuf", self.wbuf)
        view = wb.ap[:, :kc * pw].rearrange("p (k n) -> p k n", k=kc)
        src = self.wsc[(k, l)][pn]
        self.S.dma("sp", lambda e: e.dma_start(out=view, in_=src), [self.wsc_t[(k, l)][pn]], [wb])
        return wb, view

    def setup(self):
        S, nc = self.S, self.nc
        S.dma("sp", lambda e: e.dma_start(out=self.cst.ap, in_=self.cst_d), (), [self.cst])
        self.copy("dve", self.identb.ap, self.cst[:, 0:128], [self.cst], [self.identb])
        self.memset("pool", self.onesb, self.onesb.ap, 1.0)
        for h in range(8):
            self.memset("pool", self.qiZ[h], self.qiZ[h].ap, 0.0)
        self.memset("pool", self.Vsb, self.Vsb.ap, 1.0)
        cin = self.small
        S.dma("sp", lambda e: e.dma_start(out=cin[:, 0:8], in_=self.cT), (), [cin])
        self.act(self.sc_b.ap, cin[:, 0:8], AF.Silu, [cin], [self.sc_b])
        for l in range(DEPTH):
            v = self.vec[l]
            S.dma("sp", lambda e, l=l, v=v: e.dma_start(out=v.ap, in_=self.vecd[l]), (), [v])
            S.dma("pool", lambda e, l=l: e.dma_start(out=self.wwi[l].ap.rearrange("p (k n) -> p k n", k=8),
                                                       in_=self.wwi_d[l].rearrange("(kc p) n -> p kc n", p=128)), (), [self.wwi[l]])
            S.dma("pool", lambda e, l=l: e.dma_start(out=self.bd[l].ap.rearrange("p (a n) -> p a n", a=8),
                                                       in_=self.bd_d[l].rearrange("r c p n -> p (r c) n")), (), [self.bd[l]])
            for g in range(4):
                S.dma("sp", lambda e, l=l, g=g: e.dma_start(out=self.bbc[l][:, g * 128:(g + 1) * 128],
                                                             in_=self.bs_d[l, g, :].partition_broadcast(128)), (), [self.bbc[l]])
            for st_ in (self.lxh[l], self.lst[l], self.zh[l]):
                self.memset("pool", st_, st_.ap, 0.0)
        for l in range(DEPTH):
            self.precast(l)
        for l in range(DEPTH):
            v = self.vec[l]
            wtmp = self.scr[0]
            S.dma("sp", lambda e, l=l, wtmp=wtmp: e.dma_start(out=wtmp.ap.rearrange("p (g n) -> p g n", g=4),
                                                               in_=self.wsT_d[l].rearrange("g p n -> p g n")), (), [wtmp])
            self.tt("dve", self.wsm[l].ap.rearrange("p (g n) -> p g n", g=4), wtmp.ap.rearrange("p (g n) -> p g n", g=4),
                    self.cst[:, 128:256].unsqueeze(1).broadcast_to([128, 4, 128]), ALU.mult, [wtmp, self.cst], [self.wsm[l]])
            psM = self.next_ps()
            for pn in range(12):
                wb, view = self.panel("wm", l, pn)
                for cc in range(4):
                    col = pn * 4 + cc
                    for kc in range(KC):
                        self.mm(psM[:, col:col + 1], view[:, kc, cc * 128:(cc + 1) * 128], self.sc_b[:, kc:kc + 1],
                                kc == 0, kc == KC - 1, [wb, self.sc_b], [psM])
            mod = self.mod[l]
            self.tt("dve", mod.ap, psM[:, 0:48], v[:, 0:48], ALU.add, [psM, v], [mod])
            tmp = self.small
            self.ts("dve", tmp[:, 16:24], mod[:, 8:16], 1.0, None, ALU.add, None, [mod], [tmp])
            self.tt("dve", self.A1[l].ap, tmp[:, 16:24], v[:, 48:56], ALU.mult, [tmp, v], [self.A1[l]])
            self.tt("dve", self.G1[l].ap, mod[:, 16:24], v[:, 56:64], ALU.mult, [mod, v], [self.G1[l]])
            self.ts("dve", tmp[:, 24:32], mod[:, 32:40], 1.0, None, ALU.add, None, [mod], [tmp])
            self.tt("dve", self.A2[l].ap, tmp[:, 24:32], v[:, 64:72], ALU.mult, [tmp, v], [self.A2[l]])
            self.tt("dve", self.G2[l].ap, mod[:, 40:48], v[:, 72:80], ALU.mult, [mod, v], [self.G2[l]])
            self.act(tmp[:, 32:36], v[:, 136:140], AF.Exp, [v], [tmp], scale=-1.0)
            self.act(tmp[:, 36:40], tmp[:, 32:36], AF.Ln, [tmp], [tmp], bias=1.0)
            self.ts("dve", self.nsp[l][:, 0:4], tmp[:, 36:40], -8.0, None, ALU.mult, None, [tmp], [self.nsp[l]])
            self.ts("dve", self.nsp[l][:, 4:8], tmp[:, 36:40], -16.0, None, ALU.mult, None, [tmp], [self.nsp[l]])

    def prenorm(self, l, Acol, shcol):
        psN = self.psN
        for j in range(KC):
            sq = self.rot("sqb", self.sqb)
            self.act(sq.ap, self.x[j].ap, AF.Square, [self.x[j]], [sq])
            self.mm(psN.ap, self.onesb.ap, sq.ap, j == 0, j == KC - 1, [self.onesb, sq], [psN])
        self.rstd_from(psN)
        for j in range(KC):
            t = self.rot("scr", self.scr)
            self.tt("dve", t.ap, self.x[j].ap, self.rstd.ap, ALU.mult, [self.x[j], self.rstd], [t])
            self.act(self.hT[j].ap, t.ap, AF.Identity, [t, self.Aown, self.shown], [self.hT[j]], scale=Acol[:, j:j + 1], bias=shcol[:, j:j + 1])

    def rstd_from(self, psN):
        t = self.rot("scr", self.scr)
        self.act(t.ap, psN.ap, AF.Sqrt, [psN], [t], scale=1.0 / D, bias=EPS)
        self.S.op("dve", lambda e: e.reciprocal(out=self.rstd.ap, in_=t.ap), [t], [self.rstd])

    def postnorm_residual(self, Gt):
        for j in range(KC):
            t = self.rot("scr", self.scr)
            self.tt("dve", t.ap, self.o[j].ap, self.rstd.ap, ALU.mult, [self.o[j], self.rstd], [t])
            self.stt(self.x[j].ap, t.ap, Gt[:, j:j + 1], self.x[j].ap, ALU.mult, ALU.add, [t, Gt, self.x[j]], [self.x[j]])

    def rope_epi(self, psa, psb_, ci, dests):
        C, Sn = self.rope[ci], self.rope[ci + 1]
        t1 = self.rot("scr", self.scr)
        t2 = self.rot("scr", self.scr)
        self.tt("dve", t1.ap, psa.ap, C.ap, ALU.mult, [psa, C], [t1])
        self.tt("dve", t2.ap, psb_.ap, Sn.ap, ALU.mult, [psb_, Sn], [t2])
        for (dtile, dap, p0, p1) in dests:
            self.tt("pool", dap, t1[p0:p1, :], t2[p0:p1, :], ALU.add, [t1, t2], [dtile])

    def phaseA(self, l, sb):
        S = self.S
        t0 = sb * T
        v = self.vec[l]
        self.Aown, self.shown = self.A1[l], self.mod[l]
        self.prenorm(l, self.A1[l], self.mod[l][:, 0:8])
        hT = self.hT
        dbg = (sb == 0)
        if dbg:
            self.dump("hT0", hT[0], dtype=BF16)
            self.dump("rstd", self.rstd)

        def proj(view, wb, cc):
            ps = self.next_ps()
            for kc in range(KC):
                self.mm(ps.ap, view[:, kc, cc * 128:(cc + 1) * 128], hT[kc].ap, kc == 0, kc == KC - 1, [wb, hT[kc]], [ps])
            return ps

        kT3 = self.kTsb.ap.rearrange("p (h n) -> p h n", h=4)
        for pn in range(10):
            wb, view = self.panel("wF" if pn < 9 else "wF9", l, pn if pn < 9 else 0)
            if pn == 0:
                for cc in range(4):
                    ps = proj(view, wb, cc)
                    self.act(self.u[cc].ap, ps.ap, AF.Gelu_apprx_tanh, [ps], [self.u[cc]])
            elif pn in (1, 2, 3, 4, 5, 6):
                for pr in range(2):
                    psa = proj(view, wb, 2 * pr)
                    psw = proj(view, wb, 2 * pr + 1)
                    idx = ((pn - 1) % 2) * 2 + pr
                    if pn in (1, 2):
                        self.rope_epi(psa, psw, 0, [(self.qT[idx], self.qT[idx].ap, 0, 128)])
                    elif pn in (3, 4):
                        self.rope_epi(psa, psw, 0, [(self.kTsb, kT3[:, idx, :], 0, 128)])
                    else:
                        self.rope_epi(psa, psw, 2, [(self.qiZ[2 * idx], self.qiZ[2 * idx][0:64, :], 0, 64),
                                                    (self.qiZ[2 * idx + 1], self.qiZ[2 * idx + 1][64:128, :], 64, 128)])
            elif pn == 7:
                for cc in range(4):
                    ps = proj(view, wb, cc)
                    self.copy("act", self.lxb[cc][:, 3:3 + T], ps.ap, [ps], [self.lxb[cc]])
            elif pn == 8:
                for cc in range(4):
                    ps = proj(view, wb, cc)
                    self.act(self.glg[cc].ap, ps.ap, AF.Gelu_apprx_tanh, [ps], [self.glg[cc]])
            else:
                psa = proj(view, wb, 0)
                psw = proj(view, wb, 1)
                self.rope_epi(psa, psw, 2, [(self.kiTsb, self.kiTsb.ap, 0, 128)])

        wbg, vg = self.panel("wgv", l, 0)
        wbv, vv = self.panel("wv", l, 0)
        V3 = self.Vsb.ap.rearrange("p (h n) -> p h n", h=4)

        def tm_gv(s):
            ps = self.next_ps()
            for kc in range(KC):
                self.mm(ps.ap, hT[kc][:, s * 128:(s + 1) * 128], vg[:, kc, :], kc == 0, kc == KC - 1, [wbg, hT[kc]], [ps])
            gvf = self.rot("scr", self.scr)
            self.act(gvf.ap, ps.ap, AF.Gelu_apprx_tanh, [ps], [gvf])
            st6 = self.st6
            S.op("dve", lambda e, gvf=gvf: e.bn_stats(out=st6[:, 0:6], in_=gvf.ap), [gvf], [st6])
            S.op("dve", lambda e: e.bn_aggr(out=st6[:, 6:8], in_=st6[:, 0:6]), [st6], [st6])
            sm = self.small
            self.act(sm[:, 40:41], st6[:, 7:8], AF.Sqrt, [st6], [sm], bias=EPS)
            S.op("dve", lambda e: e.reciprocal(out=sm[:, 41:42], in_=sm[:, 40:41]), [sm], [sm])
            self.ts("dve", self.vh[s].ap, gvf.ap, st6[:, 6:7], sm[:, 41:42], ALU.subtract, ALU.mult, [gvf, st6, sm], [self.vh[s]])

        def tm_v(s):
            ps = self.next_ps()
            for kc in range(KC):
                self.mm(ps.ap, hT[kc][:, s * 128:(s + 1) * 128], vv[:, kc, :], kc == 0, kc == KC - 1, [wbv, hT[kc]], [ps])
            self.copy("act", V3[:, :, 0:128], ps.ap.rearrange("p (h n) -> p h n", h=4), [ps], [self.Vsb])
            blk = 4 * sb + s
            S.dma("sp", lambda e, blk=blk, l=l: e.dma_start(out=self.Vc[l][blk], in_=self.Vsb.ap), [self.Vsb], [self.Vc_t[l][blk]])
            ps2 = self.next_ps()
            w3 = self.wwi[l].ap.rearrange("p (k n) -> p k n", k=8)
            for kc in range(KC):
                self.mm(ps2[:, 0:8], hT[kc][:, s * 128:(s + 1) * 128], w3[:, kc, :], kc == 0, kc == KC - 1, [self.wwi[l], hT[kc]], [ps2])
            self.copy("dve", self.wiS[s].ap, ps2[:, 0:8], [ps2], [self.wiS[s]])

        bd3 = self.bd[l].ap.rearrange("p (a n) -> p a n", a=8)

        def lru_pre(cc):
            lxb = self.lxb[cc]
            lxh = self.lxh[l]
            self.copy("pool", lxb[:, 0:3], lxh[:, cc * 3:(cc + 1) * 3], [lxh], [lxb])
            xa = self.rot("scr", self.scr)
            self.act(xa.ap, lxb[:, 3:3 + T], AF.Identity, [lxb, v], [xa], scale=v[:, 108 + 4 * cc + 3:108 + 4 * cc + 4], bias=v[:, 124 + cc:125 + cc])
            cur = xa
            for j in (2, 1, 0):
                nx = self.rot("scr", self.scr)
                self.stt(nx.ap, lxb[:, j:j + T], v[:, 108 + 4 * cc + j:108 + 4 * cc + j + 1], cur.ap, ALU.mult, ALU.add, [lxb, v, cur], [nx])
                cur = nx
            xc = cur
            self.copy("pool", lxh[:, cc * 3:(cc + 1) * 3], lxb[:, T:T + 3], [lxb], [lxh])
            self.copy("pool", self.xcb.ap, xc.ap, [xc], [self.xcb])
            return xc

        def lru_post(cc, xc):
            v_ = v
            psr = self.next_ps()
            self.mm(psr.ap, bd3[:, cc, :], self.xcb.ap, True, True, [self.bd[l], self.xcb], [psr])
            psi = self.next_ps()
            self.mm(psi.ap, bd3[:, 4 + cc, :], self.xcb.ap, True, True, [self.bd[l], self.xcb], [psi])
            r = self.rot("scr", self.scr)
            self.act(r.ap, psr.ap, AF.Sigmoid, [psr, v], [r], bias=v[:, 128 + cc:129 + cc])
            ig = self.rot("scr", self.scr)
            self.act(ig.ap, psi.ap, AF.Sigmoid, [psi, v], [ig], bias=v[:, 132 + cc:133 + cc])
            a = self.rot("scr", self.scr)
            self.act(a.ap, r.ap, AF.Exp, [r, self.nsp[l]], [a], scale=self.nsp[l][:, cc:cc + 1])
            a2 = self.rot("scr", self.scr)
            self.act(a2.ap, r.ap, AF.Exp, [r, self.nsp[l]], [a2], scale=self.nsp[l][:, 4 + cc:5 + cc])
            self.act(a2.ap, a2.ap, AF.Sqrt, [a2], [a2], scale=-1.0, bias=1.0)
            self.tt("dve", ig.ap, ig.ap, a2.ap, ALU.mult, [ig, a2], [ig])
            self.tt("dve", ig.ap, ig.ap, xc.ap, ALU.mult, [ig, xc], [ig])
            hs = r
            lst = self.lst[l]
            S.op("dve", lambda e, hs=hs, a=a, ig=ig, lst=lst, cc=cc: e.tensor_tensor_scan(
                out=hs.ap, data0=a.ap, data1=ig.ap, initial=lst[:, cc:cc + 1], op0=ALU.mult, op1=ALU.add), [a, ig, lst], [hs])
            self.copy("dve", lst[:, cc:cc + 1], hs[:, T - 1:T], [hs], [lst])
            self.tt("pool", self.yc[cc].ap, hs.ap, self.glg[cc].ap, ALU.mult, [hs, self.glg[cc]], [self.yc[cc]])

        for i_ in range(4):
            xc_ = lru_pre(i_)
            tm_gv(i_)
            tm_v(i_)
            lru_post(i_, xc_)

        ws3 = self.wsm[l].ap.rearrange("p (g n) -> p g n", g=4)
        bb3 = self.bbc[l].ap.rearrange("p (g n) -> p g n", g=4)
        for g in range(4):
            ps = self.next_ps()
            for s in range(4):
                self.mm(ps[:, s * 128:(s + 1) * 128], self.vh[s][:, g * 128:(g + 1) * 128], ws3[:, g, :], True, True,
                        [self.vh[s], self.wsm[l]], [ps])
            t = self.rot("scr", self.scr)
            self.stt(t.ap.rearrange("p (s n) -> p s n", s=4), ps.ap.rearrange("p (s n) -> p s n", s=4), v[:, 104 + g:105 + g],
                     bb3[:, g, :].unsqueeze(1).broadcast_to([128, 4, 128]), ALU.mult, ALU.add, [ps, v, self.bbc[l]], [t])
            self.tt("pool", self.ya[g].ap, t.ap, self.u[g].ap, ALU.mult, [t, self.u[g]], [self.ya[g]])

        if dbg:
            self.dump("u0", self.u[0], dtype=BF16); self.dump("qT0", self.qT[0], dtype=BF16); self.dump("kTsb", self.kTsb, dtype=BF16)
            self.dump("qiZ0", self.qiZ[0], dtype=BF16); self.dump("qiZ1", self.qiZ[1], dtype=BF16); self.dump("kiT", self.kiTsb, dtype=BF16)
            self.dump("wiS0", self.wiS[0]); self.dump("vh0", self.vh[0], dtype=BF16); self.dump("ya0", self.ya[0], dtype=BF16)
            self.dump("yc0", self.yc[0], dtype=BF16); self.dump("Vsb", self.Vsb, dtype=BF16); self.dump("glg0", self.glg[0], dtype=BF16)
        S.dma("sp", lambda e: e.dma_start(out=self.kTc[l][:, :, t0:t0 + T].rearrange("h p n -> p h n"), in_=kT3), [self.kTsb], [self.kTc_t[l][sb]])
        S.dma("sp", lambda e: e.dma_start(out=self.kic[l][:, t0:t0 + T], in_=self.kiTsb.ap), [self.kiTsb], [self.kic_t[l][sb]])

    def attention(self, l, sb, s):
        S = self.S
        i = 4 * sb + s
        nk = i + 1
        ncol = nk * 128
        qs = slice(s * 128, (s + 1) * 128)
        wi = self.wiS[s]
        score = self.score
        ngrp = (ncol + 2047) // 2048
        for kg in range(ngrp):
            c0 = kg * 2048
            cend = min((sb + 1) * T, c0 + 2048)
            kig = self.rot("kig", self.kig)
            sbs = list(range(c0 // T, cend // T))
            S.dma("sp", lambda e, kig=kig, c0=c0, cend=cend: e.dma_start(out=kig[:, 0:cend - c0], in_=self.kic[l][:, c0:cend]),
                  [self.kic_t[l][g] for g in sbs], [kig])
            for hh in range(8):
                for c in range(c0, min(ncol, c0 + 2048), T):
                    w = min(T, ncol - c)
                    ps = self.next_ps()
                    self.mm(ps[:, :w], self.qiZ[hh][:, qs], kig[:, c - c0:c - c0 + w], True, True, [self.qiZ[hh], kig], [ps])
                    rt = self.rot("relu", self.relu)
                    self.act(rt[:, :w], ps[:, :w], AF.Relu, [ps], [rt])
                    blk = self.scoreblk[c // T]
                    if hh == 0:
                        self.ts("dve", blk[:, :w], rt[:, :w], wi[:, 0:1], None, ALU.mult, None, [rt, wi], [blk])
                    else:
                        self.stt(blk[:, :w], rt[:, :w], wi[:, hh:hh + 1], blk[:, :w], ALU.mult, ALU.add, [rt, wi, blk], [blk])
        sm = self.small
        mask = self.mask
        if ncol > self.topk:
            S.op("dve", lambda e: e.tensor_reduce(out=sm[:, 49:50], in_=score[:, :ncol], axis=AX.X, op=ALU.max, apply_absolute_value=True), [score], [sm])
        S.op("dve", lambda e: e.memset(score[0:64, ncol - 64:ncol], NEG), (), [score])
        if ncol > self.topk:
            self.ts("dve", sm[:, 48:49], sm[:, 49:50], -1.0, None, ALU.mult, None, [sm], [sm])
            self.ts("dve", sm[:, 50:51], sm[:, 49:50], 2.0, None, ALU.mult, None, [sm], [sm])
            steps = self.st_steps
            self.ts("dve", steps.ap, self.cst[:, 256:256 + NIT + 1], sm[:, 50:51], None, ALU.mult, None, [self.cst, sm], [steps])
            mid = self.midt
            self.ts("dve", mid.ap, sm[:, 48:49], steps[:, 0:1], None, ALU.add, None, [sm, steps], [mid])
            h1 = max(128, (int(ncol * 0.41) // 128) * 128)
            nB = ncol - h1
            ub0 = mask.lo
            mA = S.view("UB", ub0, ub0 + h1, mask[:, 0:h1])
            mB = S.view("UB", ub0 + h1, ub0 + ncol, mask[:, h1:ncol])
            cA, cB, t1 = self.cntA, self.cntB, self.cntT
            for k in range(NIT):
                self.ts("dve", mA.ap, score[:, 0:h1], mid[:, 0:1], 0.0, ALU.is_ge, ALU.add, [score, mid], [mA, cA], accum_out=cA[:, 0:1])
                self.act(mB.ap, score[:, h1:ncol], AF.Sign, [score, mid], [mB, cB], bias=mid[:, 0:1], scale=-1.0, accum_out=cB[:, 0:1])
                self.stt(t1[:, 0:1], cB[:, 0:1], -0.5, cA[:, 0:1], ALU.mult, ALU.add, [cA, cB], [t1])
                self.ts("dve", t1[:, 1:2], t1[:, 0:1], float(self.topk) - nB / 2.0, 0.5, ALU.is_ge, ALU.subtract, [t1], [t1])
                self.stt(mid.ap, t1[:, 1:2], steps[:, k:k + 1], mid.ap, ALU.mult, ALU.add, [t1, steps, mid], [mid])
            self.ts("dve", mask[:, :ncol], score[:, :ncol], steps[:, NIT:NIT + 1], mid[:, 0:1], ALU.add, ALU.is_ge, [score, steps, mid], [mask])
        else:
            self.ts("dve", mask[:, :ncol], score[:, :ncol], -1.0e29, None, ALU.is_ge, None, [score], [mask])
        if sb == 0:
            self.dump(f"score{s}", score, score[:, :ncol]); self.dump(f"mask{s}", mask, mask[:, :ncol], dtype=BF16)
        psb = self.psb
        mt = None
        scale = 1.0 / math.sqrt(128.0)
        psO = self.psO
        tiles = []
        for g in range(sb + 1):
            ntile = 4 if g < sb else s + 1
            for jj in range(ntile):
                tiles.append((g, jj, ntile))
        state = {}

        def emit_st(t):
            g, jj, ntile = tiles[t]
            j = 4 * g + jj
            nonlocal mt
            if jj == 0:
                if g % 2 == 0:
                    j0 = 4 * g
                    n = min(8, nk - j0)
                    for jx in range(j0, j0 + n):
                        self.tr(psb[:, (jx - j0) * 128:(jx - j0 + 1) * 128], mask[:, jx * 128:(jx + 1) * 128], [mask], [psb])
                    mt = self.rot("maskTg", self.maskTg)
                    self.copy("act", mt[:, :n * 128], psb[:, :n * 128], [psb], [mt])
                kTg = self.rot("kTg", self.kTg)
                Vg = self.rot("Vg", self.Vg)
                k3 = kTg.ap.rearrange("p (h n) -> p h n", h=4)
                V3 = Vg.ap.rearrange("p (j n) -> p j n", j=4)
                S.dma("sp", lambda e, k3=k3, g=g: e.dma_start(out=k3, in_=self.kTc[l][:, :, g * T:(g + 1) * T].rearrange("h p n -> p h n")),
                      [self.kTc_t[l][g]], [kTg])
                S.dma("sp", lambda e, V3=V3, g=g, ntile=ntile: e.dma_start(out=V3[:, 0:ntile, :], in_=self.Vc[l][4 * g:4 * g + ntile].rearrange("j p n -> p j n")),
                      [self.Vc_t[l][4 * g + jj_] for jj_ in range(ntile)], [Vg])
                state["grp"] = (kTg, Vg, k3, V3)
            kTg, Vg, k3, V3 = state["grp"]
            ps = self.next_ps()
            for h in range(4):
                self.mm(ps[:, h * 128:(h + 1) * 128], k3[:, h, jj * 128:(jj + 1) * 128], self.qT[h][:, qs], True, True,
                        [kTg, self.qT[h]], [ps])
            pe_ = self.rot("pexp", self.pexp)
            self.act(pe_.ap, ps.ap, AF.Exp, [ps], [pe_], scale=scale)
            pm = self.rot("pm", self.pm)
            mcol = (j % 8) * 128
            self.tt("dve", pm.ap.rearrange("p (h n) -> p h n", h=4), pe_.ap.rearrange("p (h n) -> p h n", h=4),
                    mt[:, mcol:mcol + 128].unsqueeze(1).broadcast_to([128, 4, 128]), ALU.mult, [pe_, mt], [pm])
            state[t] = (pm, Vg, V3, jj, j)

        def emit_pv(t):
            pm, Vg, V3, jj, j = state.pop(t)
            for h in range(4):
                po = psO[h // 2]
                c0 = (h % 2) * 129
                self.mm(po[:, c0:c0 + 129], pm[:, h * 128:(h + 1) * 128], V3[:, jj, h * 129:(h + 1) * 129],
                        (j == 0 and h % 2 == 0), j == nk - 1, [pm, Vg], [po], skip=True)

        LOOK = 2
        for t in range(min(LOOK, len(tiles))):
            emit_st(t)
        for t in range(len(tiles)):
            if t + LOOK < len(tiles):
                emit_st(t + LOOK)
            emit_pv(t)
        onb = self.onb
        for h in range(4):
            po = psO[h // 2]
            c0 = (h % 2) * 129
            S.op("dve", lambda e, po=po, c0=c0, h=h: e.reciprocal(out=sm[:, 56 + h:57 + h], in_=po[:, c0 + 128:c0 + 129]), [po], [sm])
            self.ts("dve", onb[:, h * 128:(h + 1) * 128], po[:, c0:c0 + 128], sm[:, 56 + h:57 + h], None, ALU.mult, None, [po, sm], [onb])
        for h in range(4):
            self.tr(psb[:, h * 128:(h + 1) * 128], onb[:, h * 128:(h + 1) * 128], [onb], [psb])
        self.copy("act", self.yb.ap.rearrange("p (h n) -> p h n", h=4)[:, :, qs], psb[:, 0:512].rearrange("p (h n) -> p h n", h=4),
                  [psb], [self.yb])

    def phaseB(self, l, sb):
        S = self.S
        v = self.vec[l]
        hT = self.hT
        psN = self.psN
        yb3 = self.yb.ap.rearrange("p (h n) -> p h n", h=4)
        ysrc = [(self.ya[g], self.ya[g].ap) for g in range(4)] + [(self.yb, yb3[:, h, :]) for h in range(4)] + \
               [(self.yc[g], self.yc[g].ap) for g in range(4)]
        bview = None
        for m in range(8):
            wbg, gview = self.panel("wg", l, m)
            if m % 2 == 0:
                wbb, bview = self.panel("wb", l, m // 2)
            acc = self.rot("scr", self.scr)
            for xg in range(3):
                psg = self.next_ps()
                for kc in range(KC):
                    self.mm(psg.ap, gview[:, kc, xg * 128:(xg + 1) * 128], hT[kc].ap, kc == 0, kc == KC - 1, [wbg, hT[kc]], [psg])
                gt = self.rot("scr", self.scr)
                self.act(gt.ap, psg.ap, AF.Sigmoid, [psg, v], [gt], bias=v[:, 80 + 3 * m + xg:81 + 3 * m + xg])
                psp = self.next_ps()
                for kc in range(4):
                    yt, yap = ysrc[xg * 4 + kc]
                    self.mm(psp.ap, bview[:, xg * 4 + kc, (m % 2) * 128:(m % 2 + 1) * 128], yap, kc == 0, kc == 3, [wbb, yt], [psp])
                if xg == 0:
                    self.tt("dve", acc.ap, gt.ap, psp.ap, ALU.mult, [gt, psp], [acc])
                else:
                    self.tt("dve", gt.ap, gt.ap, psp.ap, ALU.mult, [gt, psp], [gt])
                    if xg == 1:
                        self.tt("pool", acc.ap, acc.ap, gt.ap, ALU.add, [acc, gt], [acc])
                    else:
                        self.tt("pool", self.mg[m].ap, acc.ap, gt.ap, ALU.add, [acc, gt], [self.mg[m]])
        if sb == 0:
            self.dump("yb", self.yb, dtype=BF16); self.dump("mg0", self.mg[0], dtype=BF16)
        for pn in range(2):
            wb, view = self.panel("wo", l, pn)
            for cc in range(4):
                m = pn * 4 + cc
                ps = self.next_ps()
                for kc in range(KC):
                    self.mm(ps.ap, view[:, kc, cc * 128:(cc + 1) * 128], self.mg[kc].ap, kc == 0, kc == KC - 1, [wb, self.mg[kc]], [ps])
                self.copy("act", self.o[m].ap, ps.ap, [ps], [self.o[m]])
                sq = self.rot("sqb", self.sqb)
                self.act(sq.ap, ps.ap, AF.Square, [ps], [sq])
                self.mm(psN.ap, self.onesb.ap, sq.ap, m == 0, m == 7, [self.onesb, sq], [psN])
        self.rstd_from(psN)
        self.postnorm_residual(self.G1[l])
        if sb == 0:
            self.dump("x0_mid", self.x[0]); self.dump("o0_mid", self.o[0])
        self.Aown, self.shown = self.A2[l], self.mod[l]
        self.prenorm(l, self.A2[l], self.mod[l][:, 24:32])
        zh = self.zh[l]
        for pn in range(11):
            wb, view = self.panel("wu", l, pn)
            for pr in range(2):
                c = pn * 2 + pr
                cv = []
                for half in range(2):
                    q = 2 * c + half
                    ps = self.next_ps()
                    for kc in range(KC):
                        self.mm(ps.ap, view[:, kc, (2 * pr + half) * 128:(2 * pr + half + 1) * 128], hT[kc].ap, kc == 0, kc == KC - 1,
                                [wb, hT[kc]], [ps])
                    zb = self.rot("zb", self.zb)
                    self.copy("pool", zb[:, 0:2], zh[:, 2 * q:2 * q + 2], [zh], [zb])
                    self.copy("act", zb[:, 2:2 + T], ps.ap, [ps], [zb])
                    cg = self.rot("scr", self.scr)
                    wq = 140 + 3 * q
                    self.act(cg.ap, ps.ap, AF.Identity, [ps, v], [cg], scale=v[:, wq + 2:wq + 3], bias=v[:, 272 + q:273 + q])
                    self.copy("pool", zh[:, 2 * q:2 * q + 2], zb[:, T:T + 2], [zb], [zh])
                    c1 = self.rot("scr", self.scr)
                    self.stt(c1.ap, zb[:, 1:1 + T], v[:, wq + 1:wq + 2], cg.ap, ALU.mult, ALU.add, [zb, v, cg], [c1])
                    c2 = self.rot("scr", self.scr)
                    self.stt(c2.ap, zb[:, 0:T], v[:, wq:wq + 1], c1.ap, ALU.mult, ALU.add, [zb, v, c1], [c2])
                    cv.append(c2)
                self.act(cv[0].ap, cv[0].ap, AF.Silu, [cv[0]], [cv[0]])
                self.tt("pool", self.actb[c].ap, cv[0].ap, cv[1].ap, ALU.mult, [cv[0], cv[1]], [self.actb[c]])
        for m in range(8):
            wb, view = self.panel("wd", l, m)
            ps = self.next_ps()
            for kc in range(NPAIR):
                self.mm(ps.ap, view[:, kc, :], self.actb[kc].ap, kc == 0, kc == NPAIR - 1, [wb, self.actb[kc]], [ps])
            self.copy("act", self.o[m].ap, ps.ap, [ps], [self.o[m]])
            sq = self.rot("sqb", self.sqb)
            self.act(sq.ap, ps.ap, AF.Square, [ps], [sq])
            self.mm(psN.ap, self.onesb.ap, sq.ap, m == 0, m == 7, [self.onesb, sq], [psN])
        self.rstd_from(psN)
        if sb == 0:
            self.dump("act0", self.actb[0], dtype=BF16); self.dump("o0_ffn", self.o[0])
        self.postnorm_residual(self.G2[l])
        if sb == 0:
            self.dump("x0_l0", self.x[0])

    def build(self):
        S = self.S
        self.declare()
        self.st_steps = S.sb("steps", [128, NIT + 1], F32)
        self.setup()
        for sb in range(self.NSB):
            t0 = sb * T
            for j in range(KC):
                S.dma("sp", lambda e, j=j, t0=t0: e.dma_start(out=self.x[j].ap, in_=self.xT[j * 128:(j + 1) * 128, t0:t0 + T]), (), [self.x[j]])
            for i in range(4):
                S.dma("sp", lambda e, i=i, t0=t0: e.dma_start(out=self.rope[i].ap, in_=self.rope_d[i][:, t0:t0 + T]), (), [self.rope[i]])
            for l in range(DEPTH):
                self.cur_l = l
                self.phaseA(l, sb)
                for s in range(4):
                    self.attention(l, sb, s)
                self.phaseB(l, sb)
            for j in range(KC):
                S.dma("sp", lambda e, j=j, t0=t0: e.dma_start(out=self.outT[j * 128:(j + 1) * 128, t0:t0 + T], in_=self.x[j].ap), [self.x[j]], ())
        S.finish("sp")
        S.emit()
        return self.nc


def _pm(vv):
    vv = np.asarray(vv, np.float32)
    return np.ascontiguousarray(vv.reshape(-1, 128).T)


def _rope_tables(S_len):
    pos = np.arange(S_len, dtype=np.float32)[:, None]
    inv_a = (1.0 / (np.float32(10000.0) ** (np.arange(0, 128, 2, dtype=np.float32) / np.float32(128)))).astype(np.float32)
    inv_i = (1.0 / (np.float32(10000.0) ** (np.arange(0, 64, 2, dtype=np.float32) / np.float32(64)))).astype(np.float32)
    ang_a = (pos * inv_a[None, :]).astype(np.float32)
    ang_i = (pos * inv_i[None, :]).astype(np.float32)
    p = np.arange(128)
    ca = np.cos(ang_a)[:, p % 64].T
    sa = np.sin(ang_a)[:, p % 64].T * np.where(p < 64, -1.0, 1.0)[:, None]
    ci = np.cos(ang_i)[:, (p % 64) % 32].T
    si = np.sin(ang_i)[:, (p % 64) % 32].T * np.where((p % 64) < 32, -1.0, 1.0)[:, None]
    return np.ascontiguousarray(np.stack([ca, sa, ci, si]).astype(np.float32))


def prep_shared(inp, S_len):
    out = {}
    w_in = np.asarray(inp["w_in"], np.float32)
    cols = list(range(OFF["gu"], OFF["gu"] + 512))
    for base in (OFF["q"], OFF["k"]):
        for h in range(4):
            d = np.arange(128)
            cols += list(base + h * 128 + d)
            cols += list(base + h * 128 + (d + 64) % 128)
    for j in range(4):
        d = np.arange(128)
        hh = d // 64
        dd = d % 64
        cols += list(OFF["qi"] + j * 128 + d)
        cols += list(OFF["qi"] + j * 128 + hh * 64 + (dd + 32) % 64)
    cols += list(range(OFF["lx"], OFF["lx"] + 512))
    cols += list(range(OFF["lg"], OFF["lg"] + 512))
    out["s_wF"] = np.ascontiguousarray(w_in[:, :, cols])
    d = np.arange(64)
    kcols = list(OFF["ki"] + d) * 2 + list(OFF["ki"] + (d + 32) % 64) * 2
    out["s_wF9"] = np.ascontiguousarray(w_in[:, :, kcols])
    out["s_wgv"] = np.ascontiguousarray(w_in[:, :, OFF["gv"]:OFF["gv"] + 512])
    out["s_wv"] = np.ascontiguousarray(w_in[:, :, OFF["v"]:OFF["v"] + 512])
    out["s_wwi"] = np.ascontiguousarray(w_in[:, :, OFF["wi"]:OFF["wi"] + 8])
    wg = np.asarray(inp["w_gate"], np.float32)
    gcols = []
    for m in range(8):
        for xg in range(3):
            gcols += list(range(xg * 1024 + m * 128, xg * 1024 + (m + 1) * 128))
    out["s_wg"] = np.ascontiguousarray(wg[:, :, gcols])
    out["s_wb"] = np.ascontiguousarray(np.asarray(inp["w_branch"], np.float32).reshape(DEPTH, 1536, 1024))
    out["s_wo"] = np.ascontiguousarray(np.asarray(inp["w_o"], np.float32))
    wu = np.asarray(inp["ffn_w_up"], np.float32)
    ucols = []
    for c in range(NPAIR):
        ucols += list(range(c * 128, (c + 1) * 128)) + list(range(FFN + c * 128, FFN + (c + 1) * 128))
    out["s_wu"] = np.ascontiguousarray(wu[:, :, ucols])
    out["s_wd"] = np.ascontiguousarray(np.asarray(inp["ffn_w_down"], np.float32))
    out["s_wm"] = np.ascontiguousarray(np.asarray(inp["w_mod"], np.float32))
    bd = np.zeros((DEPTH, 2, 4, 128, 128), np.float32)
    for l in range(DEPTH):
        for r, key in enumerate(("lru_w_r", "lru_w_i")):
            w = np.asarray(inp[key], np.float32)[l]
            for cc in range(4):
                bd[l, r, cc, 0:64, 0:64] = w[2 * cc]
                bd[l, r, cc, 64:128, 64:128] = w[2 * cc + 1]
    out["s_bd"] = bd
    out["s_wsT"] = np.ascontiguousarray(np.asarray(inp["gm_w_s"], np.float32).transpose(0, 1, 3, 2))
    out["s_bs"] = np.ascontiguousarray(np.asarray(inp["gm_b_s"], np.float32))
    vec = np.zeros((DEPTH, 128, NV), np.float32)
    for l in range(DEPTH):
        g = lambda k: np.asarray(inp[k], np.float32)[l]
        vec[l, :, 0:48] = _pm(g("b_mod"))
        vec[l, :, 48:56] = _pm(g("g_pre1"))
        vec[l, :, 56:64] = _pm(g("g_post1"))
        vec[l, :, 64:72] = _pm(g("g_pre2"))
        vec[l, :, 72:80] = _pm(g("g_post2"))
        vec[l, :, 80:104] = _pm(g("b_gate")[gcols])
        vec[l, :, 104:108] = _pm(g("gm_v_g"))
        cw = g("lru_conv_w")
        for cc in range(4):
            for j in range(4):
                vec[l, :, 108 + 4 * cc + j] = cw[j, cc * 128:(cc + 1) * 128]
        vec[l, :, 124:128] = _pm(g("lru_conv_b"))
        vec[l, :, 128:132] = _pm(g("lru_b_r"))
        vec[l, :, 132:136] = _pm(g("lru_b_i"))
        vec[l, :, 136:140] = _pm(g("lru_lam"))
        fw = g("ffn_conv_w")[:, ucols]
        fb = g("ffn_conv_b")[ucols]
        for q in range(44):
            for j in range(3):
                vec[l, :, 140 + 3 * q + j] = fw[j, q * 128:(q + 1) * 128]
            vec[l, :, 272 + q] = fb[q * 128:(q + 1) * 128]
    out["vec"] = vec
    out["rope"] = _rope_tables(S_len)
    cst = np.zeros((128, 256 + NIT + 1), np.float32)
    cst[:, 0:128] = np.eye(128, dtype=np.float32)
    sp, tp = np.meshgrid(np.arange(128), np.arange(128), indexing="ij")
    cst[:, 128:256] = ((tp // 64) >= (sp // 64)).astype(np.float32)
    cst[:, 256:256 + NIT + 1] = (0.5 ** np.arange(1, NIT + 2, dtype=np.float64)).astype(np.float32)[None, :]
    out["cst"] = cst
    return out


def prep_core(inp, shared, b):
    m = dict(shared)
    m["xT"] = np.ascontiguousarray(np.asarray(inp["x"], np.float32)[b].T)
    m["cT"] = _pm(np.asarray(inp["c"], np.float32)[b])
    return m


_CACHE = {}


def get_prog(S_len, topk):
    key = (S_len, topk)
    if key not in _CACHE:
        p = Prog(S_len, topk)
        p.build()
        _CACHE[key] = p
    return _CACHE[key]


def kernel(**inputs):
    x = np.asarray(inputs["x"])
    B, S_len, _ = x.shape
    topk = min(256, S_len // 4)
    prog = get_prog(S_len, topk)
    shared = prep_shared(inputs, S_len)
    in_maps = [prep_core(inputs, shared, b) for b in range(B)]
    res = run_bass_kernel_spmd(prog.nc, in_maps, core_ids=list(range(B)))
    out = np.stack([np.ascontiguousarray(r["outT"].T) for r in res.results], axis=0)
    return out.astype(np.float32)
```
